# Optimizing a Trainium2 kernel written in Bass

```python
import math
import jax
import jax.numpy as jnp
from jax import lax
import numpy as np

D_MODEL = 1024
BATCH = 2
SEQ = 16384
DEPTH = 4

GRID_W = 64
CTX_LEN = 256
D_FF = 2816
H_RET = 4
RET_DV = D_MODEL // H_RET
RET_DK = RET_DV // 2
RET_CHUNK = 128
H_DIFF = 8
DIFF_DV = D_MODEL // H_DIFF
DIFF_DH = DIFF_DV // 2
Q_BLOCK = 128
ROPE_BASE = 10000.0
RET_DECAY_EXP0 = 5.0
EPS = 1e-6
N_MOD = 9
SPLIT_SIZES = (H_RET * RET_DK, H_RET * RET_DK, H_RET * RET_DV, H_RET * RET_DV,
               H_DIFF * 2 * DIFF_DH, H_DIFF * 2 * DIFF_DH, H_DIFF * DIFF_DV, 2 * D_MODEL)
SPLIT_POINTS = tuple(int(v) for v in np.cumsum(SPLIT_SIZES)[:-1])
IN_WIDTH = int(sum(SPLIT_SIZES))

kernel_name = "hybrid_retention_diffattn_dit_block"


def rmsnorm(x, w):
    xf = x.astype(jnp.float32)
    y = xf * lax.rsqrt(jnp.mean(xf * xf, axis=-1, keepdims=True) + EPS)
    return (y * w.astype(jnp.float32)).astype(x.dtype)


def groupnorm_heads(y, w):
    yf = y.astype(jnp.float32)
    mu = jnp.mean(yf, axis=-1, keepdims=True)
    var = jnp.mean(jnp.square(yf - mu), axis=-1, keepdims=True)
    return ((yf - mu) * lax.rsqrt(var + EPS) * w.astype(jnp.float32)).astype(y.dtype)


def modulate(x, g, shift, scale):
    return rmsnorm(x, g) * (1.0 + scale) + shift


def add_residual(x, y, g_post, gate, weight):
    return x + weight * gate * rmsnorm(y, g_post)


def swiglu(h, w_in, w_out):
    a, b = jnp.split(h @ w_in, 2, axis=-1)
    return (jax.nn.silu(a) * b) @ w_out


def rope(x, pos):
    half = x.shape[-1] // 2
    inv_freq = ROPE_BASE ** (-jnp.arange(half, dtype=jnp.float32) / half)
    ang = pos.astype(jnp.float32)[:, None] * inv_freq[None, :]
    cos = jnp.cos(ang).astype(x.dtype)
    sin = jnp.sin(ang).astype(x.dtype)
    x1, x2 = x[..., :half], x[..., half:]
    return jnp.concatenate([x1 * cos - x2 * sin, x1 * sin + x2 * cos], axis=-1)


def rope_2d(x, row, col):
    h = x.shape[-1] // 2
    return jnp.concatenate([rope(x[..., :h], row), rope(x[..., h:], col)], axis=-1)


def to_heads(t, n_heads):
    return jnp.transpose(t.reshape(t.shape[0], t.shape[1], n_heads, -1), (0, 2, 1, 3))


def to_diff_heads(t):
    return jnp.transpose(t.reshape(t.shape[0], t.shape[1], H_DIFF, 2, DIFF_DH), (0, 2, 3, 1, 4))


def retention_dir(q, k, v, log_gamma, state0):
    B, H, T, dk = k.shape
    dv = v.shape[-1]
    n = T // RET_CHUNK
    kc = k.astype(jnp.float32).reshape(B, H, n, RET_CHUNK, dk)
    vc = v.astype(jnp.float32).reshape(B, H, n, RET_CHUNK, dv)
    pos = jnp.arange(RET_CHUNK, dtype=jnp.float32)
    zeta = jnp.exp((RET_CHUNK - 1.0 - pos)[None, :] * log_gamma[:, None])
    kv = jnp.einsum('bhncd,bhnce->bhnde', kc * zeta[None, :, None, :, None], vc)
    g_chunk = jnp.exp(RET_CHUNK * log_gamma)[None, :, None, None]

    def step(r, kv_i):
        return g_chunk * r + kv_i, r

    final, before = lax.scan(step, state0.astype(jnp.float32), jnp.moveaxis(kv, 2, 0))
    if q is None:
        return None, final
    before = jnp.moveaxis(before, 0, 2)
    qc = q.astype(jnp.float32).reshape(B, H, n, RET_CHUNK, dk)
    diff = pos[:, None] - pos[None, :]
    dmat = jnp.where(diff >= 0.0, jnp.exp(jnp.maximum(diff, 0.0)[None] * log_gamma[:, None, None]), 0.0)
    scores = jnp.einsum('bhncd,bhnmd->bhncm', qc, kc) * dmat[None, :, None]
    inner = jnp.einsum('bhncm,bhnme->bhnce', scores, vc)
    xi = jnp.exp((pos + 1.0)[None, :] * log_gamma[:, None])
    cross = jnp.einsum('bhncd,bhnde->bhnce', qc, before) * xi[None, :, None, :, None]
    return (inner + cross).reshape(B, H, T, dv).astype(v.dtype), final


def diff_attend(q, k, v, lam):
    s = jnp.einsum('bhiqd,bhikd->bhiqk', q, k).astype(jnp.float32) * (DIFF_DH ** -0.5)
    p = jax.nn.softmax(s, axis=-1)
    a = p[:, :, 0] - lam * p[:, :, 1]
    return jnp.einsum('bhqk,bhke->bhqe', a.astype(v.dtype), v)


def diff_attend_blocked(q, k, v, lam):
    B, H, _, T, d = q.shape
    nb = T // Q_BLOCK
    qb = jnp.moveaxis(q.reshape(B, H, 2, nb, Q_BLOCK, d), 3, 0)
    out = lax.map(lambda qi: diff_attend(qi, k, v, lam), qb)
    return jnp.moveaxis(out, 0, 2).reshape(B, H, T, -1)


def retention_output(y, g, gn_w):
    y = groupnorm_heads(jnp.transpose(y, (0, 2, 1, 3)), gn_w.reshape(H_RET, RET_DV))
    return y.reshape(g.shape) * jax.nn.silu(g)


def diff_output(o, subln_w, lam_init):
    o = rmsnorm(jnp.transpose(o, (0, 2, 1, 3)), subln_w) * (1.0 - lam_init)
    return o.reshape(o.shape[0], o.shape[1], -1)


def branch_merge(ret_out, diff_out, gates, w_out):
    g_a, g_b = jnp.split(jax.nn.sigmoid(gates), 2, axis=-1)
    return (g_a * ret_out + g_b * diff_out) @ w_out


def token_mixer(h, hc, w_in, w_out, decay_logit, gn_w, lam_vec, subln_w, lam_init, row, col, tpos, need_ctx_out):
    B = h.shape[0]
    rq, rk, rv, rg, dq, dk, dv, gates = jnp.split(h @ w_in, SPLIT_POINTS, axis=-1)
    rqc, rkc, rvc, rgc, dqc, dkc, dvc, gates_c = jnp.split(hc @ w_in, SPLIT_POINTS, axis=-1)
    flip = lambda t: jnp.flip(t, axis=2)

    log_gamma = jax.nn.log_sigmoid(decay_logit.astype(jnp.float32))
    q_r = rope(to_heads(rq, H_RET), tpos)
    k_r = rope(to_heads(rk, H_RET), tpos) * (RET_DK ** -0.5)
    v_r = to_heads(rv, H_RET)
    q_rc = to_heads(rqc, H_RET) if need_ctx_out else None
    k_rc = to_heads(rkc, H_RET) * (RET_DK ** -0.5)
    v_rc = to_heads(rvc, H_RET)
    zero = jnp.zeros((B, H_RET, RET_DK, RET_DV), jnp.float32)
    yc_f, s_f = retention_dir(q_rc, k_rc, v_rc, log_gamma[0], zero)
    yc_b, s_b = retention_dir(None if q_rc is None else flip(q_rc), flip(k_rc), flip(v_rc), log_gamma[1], zero)
    y_f, _ = retention_dir(q_r, k_r, v_r, log_gamma[0], s_f)
    y_b, _ = retention_dir(flip(q_r), flip(k_r), flip(v_r), log_gamma[1], s_b)
    ret_out = retention_output(y_f + flip(y_b), rg, gn_w)

    lv = lam_vec.astype(jnp.float32)
    lam = jnp.exp(jnp.sum(lv[0] * lv[1])) - jnp.exp(jnp.sum(lv[2] * lv[3])) + lam_init
    q_d = rope_2d(to_diff_heads(dq), row, col)
    k_d = rope_2d(to_diff_heads(dk), row, col)
    v_d = to_heads(dv, H_DIFF)
    k_dc = to_diff_heads(dkc)
    v_dc = to_heads(dvc, H_DIFF)
    k_all = jnp.concatenate([k_dc, k_d], axis=3)
    v_all = jnp.concatenate([v_dc, v_d], axis=2)
    diff_out = diff_output(diff_attend_blocked(q_d, k_all, v_all, lam), subln_w, lam_init)

    y = branch_merge(ret_out, diff_out, gates, w_out)
    if not need_ctx_out:
        return y, None
    ret_out_c = retention_output(yc_f + flip(yc_b), rgc, gn_w)
    diff_out_c = diff_output(diff_attend(to_diff_heads(dqc), k_dc, v_dc, lam), subln_w, lam_init)
    yc = branch_merge(ret_out_c, diff_out_c, gates_c, w_out)
    return y, yc


def setup_inputs(seed: int = 0) -> dict:
    key = jax.random.key(seed)
    ks = jax.random.split(key, 15)
    f32 = jnp.float32
    nrm = lambda k, shape, s: s * jax.random.normal(k, shape, f32)
    decay0 = jnp.log(2.0 ** (RET_DECAY_EXP0 + jnp.arange(H_RET, dtype=f32)) - 1.0)
    return {
        "x": nrm(ks[0], (BATCH, SEQ, D_MODEL), 1.0),
        "c": nrm(ks[1], (BATCH, D_MODEL), 1.0),
        "ctx": nrm(ks[2], (BATCH, CTX_LEN, D_MODEL), 1.0),
        "c_ctx": nrm(ks[3], (D_MODEL,), 1.0),
        "w_ada": nrm(ks[4], (DEPTH, D_MODEL, N_MOD * D_MODEL), D_MODEL ** -0.5),
        "b_ada": nrm(ks[5], (DEPTH, N_MOD * D_MODEL), 0.02),
        "norm_w": 1.0 + nrm(ks[6], (DEPTH, 6, D_MODEL), 0.02),
        "ffn_w_in": nrm(ks[7], (DEPTH, 2, D_MODEL, 2 * D_FF), D_MODEL ** -0.5),
        "ffn_w_out": nrm(ks[8], (DEPTH, 2, D_FF, D_MODEL), D_FF ** -0.5),
        "w_in": nrm(ks[9], (DEPTH, D_MODEL, IN_WIDTH), D_MODEL ** -0.5),
        "w_out": nrm(ks[10], (DEPTH, D_MODEL, D_MODEL), D_MODEL ** -0.5),
        "ret_decay_logit": decay0[None, None, :] + nrm(ks[11], (DEPTH, 2, H_RET), 0.1),
        "ret_gn_w": 1.0 + nrm(ks[12], (DEPTH, H_RET * RET_DV), 0.02),
        "diff_lambda": nrm(ks[13], (DEPTH, 4, DIFF_DH), 0.1),
        "diff_subln_w": 1.0 + nrm(ks[14], (DEPTH, DIFF_DV), 0.02),
    }


def reference(x, c, ctx, c_ctx, w_ada, b_ada, norm_w, ffn_w_in, ffn_w_out, w_in, w_out,
              ret_decay_logit, ret_gn_w, diff_lambda, diff_subln_w):
    B, T, D = x.shape
    rows = T // GRID_W
    row = jnp.broadcast_to(jnp.arange(rows, dtype=jnp.int32)[:, None], (rows, GRID_W)).reshape(-1)
    col = jnp.broadcast_to(jnp.arange(GRID_W, dtype=jnp.int32)[None, :], (rows, GRID_W)).reshape(-1)
    tpos = jnp.arange(rows * GRID_W, dtype=jnp.int32)
    xc = ctx
    for l in range(DEPTH):
        last = l == DEPTH - 1
        lam_init = 0.8 - 0.6 * math.exp(-0.3 * l)
        mod = (jax.nn.silu(c) @ w_ada[l] + b_ada[l]).reshape(B, N_MOD, 1, D)
        mod_c = (jax.nn.silu(c_ctx) @ w_ada[l] + b_ada[l]).reshape(N_MOD, D)
        nw = norm_w[l]
        x = add_residual(x, swiglu(modulate(x, nw[0], mod[:, 0], mod[:, 1]), ffn_w_in[l, 0], ffn_w_out[l, 0]),
                         nw[1], mod[:, 2], 0.5)
        xc = add_residual(xc, swiglu(modulate(xc, nw[0], mod_c[0], mod_c[1]), ffn_w_in[l, 0], ffn_w_out[l, 0]),
                          nw[1], mod_c[2], 0.5)
        h = modulate(x, nw[2], mod[:, 3], mod[:, 4])
        hc = modulate(xc, nw[2], mod_c[3], mod_c[4])
        y, yc = token_mixer(h, hc, w_in[l], w_out[l], ret_decay_logit[l], ret_gn_w[l], diff_lambda[l],
                            diff_subln_w[l], lam_init, row, col, tpos, not last)
        x = add_residual(x, y, nw[3], mod[:, 5], 1.0)
        x = add_residual(x, swiglu(modulate(x, nw[4], mod[:, 6], mod[:, 7]), ffn_w_in[l, 1], ffn_w_out[l, 1]),
                         nw[5], mod[:, 8], 0.5)
        if not last:
            xc = add_residual(xc, yc, nw[3], mod_c[5], 1.0)
            xc = add_residual(xc, swiglu(modulate(xc, nw[4], mod_c[6], mod_c[7]), ffn_w_in[l, 1], ffn_w_out[l, 1]),
                              nw[5], mod_c[8], 0.5)
    return x
```

```python
import math
from contextlib import ExitStack
import numpy as np
import concourse.bass as bass
import concourse.mybir as mybir
from concourse.bass_utils import run_bass_kernel_spmd

F32 = mybir.dt.float32
BF16 = mybir.dt.bfloat16
AF = mybir.ActivationFunctionType
ALU = mybir.AluOpType

D = 1024
DFF = 2816
T = 16384
NT = 4096
CT = 256
NTC = NT + 2 * CT
NB = 2048
BLKV = [0, 0, 0, 0, 1, 1, 1, 1, 2, 2]
NL = 4
EPS = 1e-6
NCORES = 8
WROWS = 3648
OFF_FIN = [0, 8650752]
OFF_FOUT = [5767168, 8650752 + 5767168]
OFF_WFM = 2 * 8650752
OFF_WTM = OFF_WFM + 72 * 128 * 1024
OFF_WO = OFF_WTM + 4 * 128 * 4096
LAYER_ELEMS = OFF_WO + 8 * 128 * 1024
assert LAYER_ELEMS == 8 * WROWS * 1024

SAME_ENGINE_SYNC = True


class Ev:
    __slots__ = ("key", "sem", "val")

    def __init__(self, key, sem, val):
        self.key, self.sem, self.val = key, sem, val


class Buf:
    __slots__ = ("w", "r", "sem", "semcnt", "name", "uid", "gen")
    _n = [0]

    def __init__(self, name=""):
        Buf._n[0] += 1
        self.uid = Buf._n[0]
        self.gen = 0
        self.w = None
        self.r = {}
        self.sem = None
        self.semcnt = 0
        self.name = name


class KB:
    def __init__(self, nc, stack):
        self.nc = nc
        self.stack = stack
        self.eng = {"pe": nc.tensor, "act": nc.scalar, "dve": nc.vector, "pool": nc.gpsimd, "sp": nc.sync}
        self.psem = {n: stack.enter_context(nc.semaphore("prog_" + n)) for n in self.eng}
        self.cnt = {n: 0 for n in self.eng}
        self.seen = {n: {} for n in self.eng}
        self.pending = {n: [] for n in self.eng}
        self.dma_bufs = []
        self.free = []
        self.nsem = 0
        self.ninst = 0

    def _wait(self, me, ev):
        if ev.key == me and (me == "pe" or not SAME_ENGINE_SYNC):
            return
        assert ev.val is not None, "dependency on an unsignaled op"
        if self.seen[me].get(ev.key, 0) >= ev.val:
            return
        self.eng[me].wait_ge(ev.sem, ev.val)
        self.seen[me][ev.key] = ev.val

    def _deps(self, me, reads, writes):
        for b in reads:
            if b.w is not None:
                self._wait(me, b.w)
        for b in writes:
            if b.w is not None:
                self._wait(me, b.w)
            for e in b.r.values():
                self._wait(me, e)

    def _mark(self, ev, reads, writes):
        for b in reads:
            b.r[ev.key] = ev
        for b in writes:
            b.w = ev
            b.r = {}

    def op(self, me, fn, reads=(), writes=(), sig=True):
        self._deps(me, reads, writes)
        inst = fn(self.eng[me])
        self.ninst += 1
        if sig:
            self.cnt[me] += 1
            inst.then_inc(self.psem[me], 1)
            ev = Ev(me, self.psem[me], self.cnt[me])
            for p in self.pending[me]:
                p.val = self.cnt[me]
            self.pending[me] = []
        else:
            ev = Ev(me, self.psem[me], None)
            self.pending[me].append(ev)
        self._mark(ev, reads, writes)

    def dma(self, q, out, in_, sbuf, reads=(), writes=()):
        self._deps(q, reads, writes)
        self._getsem(sbuf)
        inst = self.eng[q].dma_start(out=out, in_=in_)
        inst.then_inc(sbuf.sem, 16)
        self.ninst += 1
        sbuf.semcnt += 16
        ev = Ev(("d", sbuf.uid, sbuf.gen), sbuf.sem, sbuf.semcnt)
        self._mark(ev, reads, writes)

    def _getsem(self, b):
        if b.sem is None:
            b.gen += 1
            if self.free:
                b.sem, b.semcnt = self.free.pop()
            else:
                b.sem = self.stack.enter_context(self.nc.semaphore("d%d" % self.nsem))
                b.semcnt = 0
                self.nsem += 1
            self.dma_bufs.append(b)

    def collective(self, in_h, out_h, cbuf, reads=(), writes=()):
        self._deps("pool", reads, writes)
        self._getsem(cbuf)
        inst = self.nc.gpsimd.collective_compute(
            "AllGather", ALU.bypass, replica_groups=[list(range(NCORES))],
            ins=[in_h.ap().opt()], outs=[out_h.ap().opt()])
        inst.then_inc(cbuf.sem)
        cbuf.semcnt += 1
        ev = Ev(("d", cbuf.uid, cbuf.gen), cbuf.sem, cbuf.semcnt)
        self._mark(ev, reads, writes)

    def barrier(self):
        for n in self.eng:
            assert not self.pending[n], "pending unsignaled ops at barrier"
        sp = self.eng["sp"]
        for n in ("pe", "act", "dve", "pool"):
            if self.cnt[n] > self.seen["sp"].get(n, 0):
                sp.wait_ge(self.psem[n], self.cnt[n])
        for b in self.dma_bufs:
            k = ("d", b.uid, b.gen)
            if b.semcnt > self.seen["sp"].get(k, 0):
                sp.wait_ge(b.sem, b.semcnt)
        self.cnt["sp"] += 1
        sp.sem_inc(self.psem["sp"], 1)
        for n in ("pe", "act", "dve", "pool"):
            self.eng[n].wait_ge(self.psem["sp"], self.cnt["sp"])
        for n in self.eng:
            for m in self.eng:
                self.seen[n][m] = self.cnt[m]
            for b in self.dma_bufs:
                self.seen[n][("d", b.uid, b.gen)] = b.semcnt
        for b in self.dma_bufs:
            self.free.append((b.sem, b.semcnt))
            b.sem = None
        self.dma_bufs = []


class Ring:
    def __init__(self, tensor, n):
        self.t = tensor
        self.n = n
        self.bufs = [Buf() for _ in range(n)]
        self.i = 0

    def next(self):
        i = self.i
        self.i = (i + 1) % self.n
        return i, self.bufs[i]


def _rope_tables():
    p = np.arange(128)
    tpos = np.arange(T, dtype=np.float32)
    invf = (10000.0 ** (-(np.arange(64, dtype=np.float32)) / 64)).astype(np.float32)
    ang = tpos[None, :] * invf[p % 64][:, None]
    rc = np.cos(ang).astype(np.float32)
    rs = np.sin(ang).astype(np.float32) * np.where(p < 64, -1.0, 1.0).astype(np.float32)[:, None]
    partner_r = np.where(p < 64, p + 64, p - 64)
    row = (np.arange(T) // 64).astype(np.float32)
    col = (np.arange(T) % 64).astype(np.float32)
    invf16 = (10000.0 ** (-(np.arange(16, dtype=np.float32)) / 16)).astype(np.float32)
    d = p % 64
    isrow = d < 32
    dd = d % 32
    f = dd % 16
    pos = np.where(isrow[:, None], row[None, :], col[None, :]).astype(np.float32)
    angd = pos * invf16[f][:, None]
    dc_ = np.cos(angd).astype(np.float32)
    ds_ = np.sin(angd).astype(np.float32) * np.where(dd < 16, -1.0, 1.0).astype(np.float32)[:, None]
    partner_d = np.where(dd < 16, p + 16, p - 16)
    return rc, rs, partner_r, dc_, ds_, partner_d


def _fm_cols(partner_r, partner_d):
    cols = []
    p = np.arange(128)
    for base, hn, partner in ((0, 4, partner_r), (512, 4, partner_r)):
        for h in range(hn):
            cols.append(base + h * 128 + p)
        for h in range(hn):
            cols.append(base + h * 128 + partner)
    for base in (3072, 4096):
        for h in range(8):
            cols.append(base + h * 128 + p)
        for h in range(8):
            cols.append(base + h * 128 + partner_d)
    for base in (2048, 6144, 7168):
        for c in range(8):
            cols.append(base + c * 128 + p)
    return np.stack(cols)
C_RQ, C_RQS, C_RK, C_RKS, C_DQ, C_DQS, C_DK, C_DKS, C_RG, C_GA, C_GB = 0, 4, 8, 12, 16, 24, 32, 40, 48, 56, 64


def _tile_w(w, ncol_tiles, colsel=None):
    K = w.shape[0]
    if colsel is not None:
        w = w[:, colsel.reshape(-1)]
    return w.reshape(K // 128, 128, ncol_tiles, -1).transpose(2, 1, 0, 3)


def prep_inputs(inp):
    f32 = np.float32
    x = np.asarray(inp["x"], f32)
    c = np.asarray(inp["c"], f32)
    ctx = np.asarray(inp["ctx"], f32)
    c_ctx = np.asarray(inp["c_ctx"], f32)
    w_ada = np.asarray(inp["w_ada"], f32)
    b_ada = np.asarray(inp["b_ada"], f32)
    norm_w = np.asarray(inp["norm_w"], f32)
    ffn_w_in = np.asarray(inp["ffn_w_in"], f32)
    ffn_w_out = np.asarray(inp["ffn_w_out"], f32)
    w_in = np.asarray(inp["w_in"], f32)
    w_out = np.asarray(inp["w_out"], f32)
    rc, rs, partner_r, dc_, ds_, partner_d = _rope_tables()
    fmcols = _fm_cols(partner_r, partner_d)
    flat = np.empty((NL, LAYER_ELEMS), f32)
    for l in range(NL):
        for f in range(2):
            wi = ffn_w_in[l, f]
            a = _tile_w(wi[:, :DFF], 22)
            b = _tile_w(wi[:, DFF:], 22)
            flat[l, OFF_FIN[f]:OFF_FIN[f] + 22 * 128 * 2048] = np.concatenate([a, b], axis=3).reshape(-1)
            flat[l, OFF_FOUT[f]:OFF_FOUT[f] + 8 * 128 * 2816] = _tile_w(ffn_w_out[l, f], 8).reshape(-1)
        flat[l, OFF_WFM:OFF_WTM] = _tile_w(w_in[l], 72, fmcols).reshape(-1)
        tmc = np.concatenate([1024 + np.arange(1024), 5120 + np.arange(1024)])
        wt = w_in[l][:, tmc].reshape(8, 128, 4, 512).transpose(2, 1, 0, 3)
        flat[l, OFF_WTM:OFF_WO] = wt.reshape(-1)
        flat[l, OFF_WO:] = _tile_w(w_out[l], 8).reshape(-1)
    flat = flat.reshape(NL, NCORES, WROWS, 1024)
    flat = np.concatenate([flat, np.zeros((NL, NCORES, 4096 - WROWS, 1024), f32)], axis=2)
    cin = np.stack([c[0], c[1], c_ctx], axis=1).reshape(8, 128, 3).transpose(1, 0, 2)
    normw = norm_w.reshape(NL, 6, 8, 128).transpose(3, 0, 1, 2)
    gnw = np.asarray(inp["ret_gn_w"], f32).reshape(NL, 8, 128).transpose(2, 0, 1)
    sublnw = np.asarray(inp["diff_subln_w"], f32).T
    dlam = np.broadcast_to(np.asarray(inp["diff_lambda"], f32).reshape(1, NL * 256), (128, NL * 256))
    decay = np.broadcast_to(np.asarray(inp["ret_decay_logit"], f32).reshape(1, NL * 8), (128, NL * 8))
    pp = np.arange(128, dtype=f32)
    cm = np.arange(128, dtype=f32)[None, :] - pp[:, None]
    misc = np.zeros((128, 8, 128), f32)
    misc[:, 0] = np.maximum(cm, 0)
    misc[:, 1] = (cm >= 0) * (128.0 ** -0.5)
    misc[:, 2] = np.maximum(-cm, 0)
    misc[:, 3] = (cm <= 0) * (128.0 ** -0.5)
    misc[:, 4] = np.arange(128, dtype=f32)[None, :] + 1.0
    misc[:, 5] = 128.0 - np.arange(128, dtype=f32)[None, :]
    misc[:, 6, 0] = 127.0 - pp
    misc[:, 6, 1] = pp
    misc[:, 7] = np.eye(128, dtype=f32)
    maps = []
    for core in range(NCORES):
        t0 = core * NB
        xT = np.concatenate([x[0, t0:t0 + NB].T, x[1, t0:t0 + NB].T], axis=1).reshape(8, 128, NT)
        ctxT = np.concatenate([ctx[0].T, ctx[1].T], axis=1).reshape(8, 128, 2 * CT)
        wada = w_ada[:, :, core * 1152:(core + 1) * 1152].reshape(NL, 8, 128, 1152).transpose(0, 2, 1, 3)
        bada = b_ada[:, core * 1152:(core + 1) * 1152].reshape(NL, 9, 128).transpose(2, 0, 1)
        ropes = np.empty((4, 128, NB + CT), f32)
        ropes[0, :, :NB] = rc[:, t0:t0 + NB]
        ropes[1, :, :NB] = rs[:, t0:t0 + NB]
        ropes[2, :, :NB] = dc_[:, t0:t0 + NB]
        ropes[3, :, :NB] = ds_[:, t0:t0 + NB]
        ropes[0::2, :, NB:] = 1.0
        ropes[1::2, :, NB:] = 0.0
        rcoef = np.zeros((128, 2, 2, 9), f32)
        for rr in range(8):
            if rr < core:
                rcoef[:, 0, 0, rr] = 2048.0 * (core - 1 - rr)
                rcoef[:, 0, 1, rr] = 1.0
            if rr > core:
                rcoef[:, 1, 0, rr] = 2048.0 * (rr - core - 1)
                rcoef[:, 1, 1, rr] = 1.0
        rcoef[:, 0, 0, 8] = 2048.0 * core
        rcoef[:, 0, 1, 8] = 1.0
        rcoef[:, 1, 0, 8] = 2048.0 * (7 - core)
        rcoef[:, 1, 1, 8] = 1.0
        maps.append({
            "xT": np.ascontiguousarray(xT), "ctxT": np.ascontiguousarray(ctxT),
            "cin": np.ascontiguousarray(cin), "wsh": np.ascontiguousarray(flat[:, core]),
            "wada": np.ascontiguousarray(wada), "bada": np.ascontiguousarray(bada),
            "normw": np.ascontiguousarray(normw), "gnw": np.ascontiguousarray(gnw),
            "sublnw": np.ascontiguousarray(sublnw), "dlam": np.ascontiguousarray(dlam),
            "decay": np.ascontiguousarray(decay), "ropes": ropes, "misc": misc, "rcoef": rcoef,

        })
    return maps


class Prog:
    def __init__(self, dbg=None, nlayers=NL, stop=None):
        self.dbg = dbg or []
        self.nlayers = nlayers
        self.stop = stop
        self.nc = bass.Bass("TRN2", target_bir_lowering=False)
        self.stack = ExitStack()

    def un(self, name):
        self._u = getattr(self, "_u", 0) + 1
        return "%s_%d" % (name, self._u)

    def dram_in(self, name, shape, dt=F32):
        return self.nc.dram_tensor(name, list(shape), dt, kind="ExternalInput")

    def dram(self, name, shape, dt, out=False):
        kind = "ExternalOutput" if (out or name in self.dbg) else "Internal"
        return self.nc.dram_tensor(name, list(shape), dt, kind=kind)

    def sb(self, name, shape, dt):
        return self.stack.enter_context(self.nc.sbuf_tensor(name, list(shape), dt))

    def build(self):
        nc = self.nc
        with self.stack:
            self.k = KB(nc, self.stack)
            self._declare()
            self._prologue()
            phases = []
            for l in range(self.nlayers):
                phases += [("ffn", l, 0), ("inproj", l), ("ret", l), ("diff", l), ("oproj", l), ("ffn", l, 1)]
            for ph in phases:
                if ph[0] == "ffn":
                    self._ffn(ph[1], ph[2], final=(ph == phases[-1] and self.stop is None))
                elif ph[0] == "inproj":
                    self._inproj(ph[1])
                elif ph[0] == "ret":
                    self._ret(ph[1], last=(ph[1] == NL - 1))
                elif ph[0] == "diff":
                    self._diff(ph[1], last=(ph[1] == NL - 1))
                elif ph[0] == "oproj":
                    self._oproj(ph[1], last=(ph[1] == NL - 1))
                if self.stop == ph:
                    break
            self.k.barrier()
        return nc

    def _declare(self):
        nc = self.nc
        self.xT = self.dram_in("xT", [8, 128, NT])
        self.ctxT = self.dram_in("ctxT", [8, 128, 2 * CT])
        self.cin = self.dram_in("cin", [128, 8, 3])
        self.wsh = self.dram_in("wsh", [NL, 4096, 1024])
        self.wada = self.dram_in("wada", [NL, 128, 8, 1152])
        self.bada = self.dram_in("bada", [128, NL, 9])
        self.normw = self.dram_in("normw", [128, NL, 6, 8])
        self.gnw = self.dram_in("gnw", [128, NL, 8])
        self.sublnw = self.dram_in("sublnw", [128, NL])
        self.dlam = self.dram_in("dlam", [128, NL * 256])
        self.decay = self.dram_in("decay", [128, NL * 8])
        self.ropes = self.dram_in("ropes", [4, 128, NB + CT])
        self.misc = self.dram_in("misc", [128, 8, 128])
        self.outT = self.dram("outT", [8, 128, NT], F32, out=True)
        self.xres = self.dram("xres", [8, 128, NTC], F32)
        self.wall = [self.dram("wall%d" % l, [8 * WROWS, 1024], BF16) for l in range(NL)]
        self.modsrc = self.dram("modsrc", [128, NL * 27], F32)
        self.modall = self.dram("modall", [8 * 128, NL * 27], F32)
        self.xres_b = [Buf("xres%d" % i) for i in range(10)]
        self.QR = self.dram("QR", [4, 128, NTC], BF16)
        self.KR = self.dram("KR", [4, 128, NTC], BF16)
        self.DQ = self.dram("DQ", [8, 128, NTC], BF16)
        self.DKs = self.dram("DKs", [2 * 8 * 128, NB], BF16)
        self.DKg = self.dram("DKg", [8, 4096, 1024], BF16)
        self.DKc = self.dram("DKc", [2, 8, 128, CT], BF16)
        self.DVs = self.dram("DVs", [2 * NB, 1024], BF16)
        self.DVg = self.dram("DVg", [8, 4096, 1024], BF16)
        self.DVc = self.dram("DVc", [2 * CT, 1024], BF16)
        self.RV = self.dram("RV", [NTC, 1024], BF16)
        self.RG = self.dram("RG", [8, 128, NTC], BF16)
        self.GA = self.dram("GA", [8, 128, NTC], BF16)
        self.GB = self.dram("GB", [8, 128, NTC], BF16)
        self.MA = self.dram("MA", [8, 128, NTC], BF16)
        self.MB = self.dram("MB", [8, 128, NTC], BF16)
        self.Asrc = self.dram("Asrc", [16 * 128, 256], F32)
        self.Aall = self.dram("Aall", [8, 1024, 1024], BF16)
        self.ccsrc = self.dram("ccsrc", [4096, 1024], BF16)
        self.ccdst = self.dram("ccdst", [8 * 4096, 1024], BF16)
        self.b_ccsrc, self.b_ccdst = Buf("ccsrc"), Buf("ccdst")
        self.sc_b = {}
        self.wall_b = [Buf("wall%d" % l) for l in range(NL)]
        self.ones_bf = self.sb("ones_bf", [128, 128], BF16)
        self.cst = self.sb("cst", [128, 8], F32)
        self.ones_f = self.sb("ones_f", [128, 128], F32)
        self.misc_sb = self.sb("misc_sb", [128, 8, 128], F32)
        self.rcoef = self.dram_in("rcoef", [128, 2, 2, 9])
        self.rcoef_sb = self.sb("rcoef_sb", [128, 2, 2, 9], F32)
        self.gnw_sb = self.sb("gnw_sb", [128, NL, 8], F32)
        self.subln_sb = self.sb("subln_sb", [128, NL], F32)
        self.dlam_sb = self.sb("dlam_sb", [128, NL * 256], F32)
        self.decay_sb = self.sb("decay_sb", [128, NL * 8], F32)
        self.modT = self.sb("modT", [128, 8, NL * 27], F32)
        self.normw_sb = self.sb("normw_sb", [128, NL, 6, 8], F32)
        self.pre = self.sb("pre", [128, NL, 3, 3, 8], F32)
        self.post = self.sb("post", [128, NL, 3, 3, 8], F32)
        self.psum = self.stack.enter_context(nc.psum_tensor("psum", [128, 8, 512], F32))
        self.pb = [Buf("ps%d" % i) for i in range(8)]
        self.pi = 0

    def allgather(self, src_ap, nrows, dst_ap, reads, writes, q="pool", nout=None):
        k = self.k
        cpb = Buf()
        k.dma("pool", self.ccsrc.ap()[0:nrows, :], src_ap, cpb, reads=list(reads), writes=[self.b_ccsrc])
        k.collective(self.ccsrc, self.ccdst, Buf(), reads=[self.b_ccsrc], writes=[self.b_ccdst])
        cview = self.ccdst.ap().rearrange("(r a) c -> r a c", r=8)
        cpb2 = Buf()
        for r in range(8):
            k.dma(q, dst_ap[r], cview[r, 0:(nout or nrows), :], cpb2, reads=[self.b_ccdst], writes=list(writes))

    def scb(self, name, blk):
        key = (name, blk)
        if key not in self.sc_b:
            self.sc_b[key] = Buf("%s%s" % (name, blk))
        return self.sc_b[key]

    def bank(self, lo=0, hi=8):
        i = lo + (self.pi % (hi - lo))
        self.pi += 1
        return i, self.pb[i]

    def wtile(self, l, off, nrow, ncol):
        flat = self.wall[l].ap().rearrange("a b -> (a b)")
        return flat[off:off + nrow * ncol].rearrange("(p k) -> p k", p=nrow)

    def modv(self, l, j, dc, v):
        ch = j * 8 + dc
        r, q = ch // 9, ch % 9
        idx = l * 27 + q * 3 + v
        return self.modT[:, r, idx:idx + 1]

    def _prologue(self):
        k, nc = self.k, self.nc
        cb = Buf("const")
        k.op("pool", lambda e: e.memset(self.ones_bf[:], 1.0), writes=[cb])
        k.op("pool", lambda e: e.memset(self.cst[:, 0:1], 1.0), writes=[cb])
        k.op("pool", lambda e: e.memset(self.cst[:, 1:2], EPS), writes=[cb])
        k.op("pool", lambda e: e.memset(self.cst[:, 2:3], 0.0), writes=[cb])
        k.op("pool", lambda e: e.memset(self.ones_f[:], 1.0), writes=[cb])
        for (dst_, src_) in ((self.misc_sb, self.misc), (self.rcoef_sb, self.rcoef), (self.gnw_sb, self.gnw),
                             (self.subln_sb, self.sublnw), (self.dlam_sb, self.dlam), (self.decay_sb, self.decay)):
            k.dma("sp", dst_[:], src_.ap(), cb, writes=[cb])
        for l in range(self.nlayers):
            self.allgather(self.wsh.ap()[l], 4096, self.wall[l].ap().rearrange("(r a) c -> r a c", r=8), [], [self.wall_b[l]], nout=WROWS)
        with ExitStack() as st:
            cin_sb = st.enter_context(nc.sbuf_tensor("cin_sb", [128, 8, 3], F32))
            sil = st.enter_context(nc.sbuf_tensor("sil", [128, 8, 3], F32))
            wa = st.enter_context(nc.sbuf_tensor("wa", [128, 2, 8, 1152], F32))
            bada_sb = st.enter_context(nc.sbuf_tensor("bada_sb", [128, NL, 9], F32))
            modl = st.enter_context(nc.sbuf_tensor("modl", [128, NL * 27], F32))
            b_cin, b_sil, b_bada, b_modl, b_nw = Buf(), Buf(), Buf(), Buf(), Buf()
            k.op("pool", lambda e: e.memset(modl[:], 0.0), writes=[b_modl])
            b_wa = [Buf(), Buf()]
            k.dma("sp", cin_sb[:], self.cin.ap(), b_cin, writes=[b_cin])
            k.dma("sp", bada_sb[:], self.bada.ap(), b_bada, writes=[b_bada])
            k.dma("sp", self.normw_sb[:], self.normw.ap(), b_nw, writes=[b_nw])
            k.op("act", lambda e: e.activation(out=sil[:], in_=cin_sb[:], func=AF.Silu), reads=[b_cin], writes=[b_sil])
            for l in range(self.nlayers):
                s = l % 2
                k.dma("sp", wa[:, s], self.wada.ap()[l], b_wa[s], writes=[b_wa[s]])
                for q in range(9):
                    bi, bb = self.bank()
                    for kc in range(8):
                        k.op("pe", lambda e: e.matmul(self.psum[:, bi, 0:3], lhsT=wa[:, s, kc, q * 128:(q + 1) * 128],
                                                      rhs=sil[:, kc, :], start=(kc == 0), stop=(kc == 7)),
                             reads=[b_wa[s], b_sil], writes=[bb], sig=(kc == 7))
                    o = l * 27 + q * 3
                    k.op("dve", lambda e: e.tensor_scalar(out=modl[:, o:o + 3], in0=self.psum[:, bi, 0:3],
                                                          scalar1=bada_sb[:, l, q:q + 1], scalar2=None, op0=ALU.add),
                         reads=[bb, b_bada], writes=[b_modl])
            b_ms = Buf()
            k.dma("sp", self.modsrc.ap(), modl[:], b_modl, reads=[b_modl], writes=[b_ms])
            b_ma = Buf()
            k.collective(self.modsrc, self.modall, Buf(), reads=[b_ms], writes=[b_ma])
            b_modT = Buf()
            k.dma("sp", self.modT[:], self.modall.ap().rearrange("(r p) n -> p r n", p=128), b_modT,
                  reads=[b_ma], writes=[b_modT])
            for l in range(self.nlayers):
                for s in range(3):
                    wgt = 1.0 if s == 1 else 0.5
                    for v in range(3):
                        for dc in range(8):
                            vv = v
                            k.op("dve", lambda e: e.tensor_scalar(
                                out=self.pre[:, l, s, v, dc:dc + 1], in0=self.modv(l, 3 * s + 1, dc, vv),
                                scalar1=1.0, scalar2=self.normw_sb[:, l, 2 * s, dc:dc + 1], op0=ALU.add, op1=ALU.mult),
                                 reads=[b_modT, b_nw], writes=[cb])
                            k.op("dve", lambda e: e.tensor_scalar(
                                out=self.post[:, l, s, v, dc:dc + 1], in0=self.modv(l, 3 * s + 2, dc, vv),
                                scalar1=wgt, scalar2=self.normw_sb[:, l, 2 * s + 1, dc:dc + 1], op0=ALU.mult, op1=ALU.mult),
                                 reads=[b_modT, b_nw], writes=[cb])
            xb = st.enter_context(nc.sbuf_tensor("xinit", [128, 2, 8, 512], F32))
            xbb = [Buf(), Buf()]
            for blk in range(10):
                s = blk % 2
                w = 512 if blk < 8 else CT
                c0 = blk * 512 if blk < 8 else NT + (blk - 8) * CT
                src = self.xT.ap()[:, :, c0:c0 + w] if blk < 8 else self.ctxT.ap()[:, :, (blk - 8) * CT:(blk - 7) * CT]
                k.dma("sp", xb[:, s, :, 0:w], src.rearrange("c p t -> p c t"), xbb[s], writes=[xbb[s]])
                k.dma("pool", self.xres.ap()[:, :, c0:c0 + w].rearrange("c p t -> p c t"), xb[:, s, :, 0:w], xbb[s],
                      reads=[xbb[s]], writes=[self.xres_b[blk]])
            k.barrier()

    def _vsel(self, v):
        return 2 if v == 1 else 0

    def _ffn(self, l, f, final=False):
        k, nc = self.k, self.nc
        s_idx = 0 if f == 0 else 2
        woff_in, woff_out = OFF_FIN[f], OFF_FOUT[f]
        with ExitStack() as st:
            def sbt(name, shape, dt):
                return st.enter_context(nc.sbuf_tensor(self.un(name), shape, dt))
            xt = sbt("f_x", [128, 2, 8, 512], F32)
            xbuf = [Buf(), Buf()]
            sq = sbt("f_sq", [128, 8, 512], BF16)
            b_sq = [Buf() for _ in range(8)]
            rstd = sbt("f_rstd", [128, 512], F32)
            b_rstd = Buf()
            tmp = sbt("f_tmp", [128, 2, 512], F32)
            tmp_r = Ring(tmp, 2)
            h = sbt("f_h", [128, 8, 512], BF16)
            b_h = [Buf() for _ in range(8)]
            hid = sbt("f_hid", [128, 22, 512], BF16)
            b_hid = [Buf() for _ in range(22)]
            sa = sbt("f_sa", [128, 2, 512], F32)
            sa_r = Ring(sa, 2)
            y = sbt("f_y", [128, 8, 512], F32)
            b_y = [Buf() for _ in range(8)]
            w1 = sbt("f_w1", [128, 4, 2048], BF16)
            w1_r = Ring(w1, 4)
            w2 = sbt("f_w2", [128, 2, 2816], BF16)
            w2_r = Ring(w2, 2)
            nblk = 10 if not (final) else 8
            for blk in range(nblk):
                v = BLKV[blk]
                W, c0 = self.blkgeom(blk)
                s = blk % 2
                xs, xb = xt[:, s], xbuf[s]
                if blk == 0:
                    self._xload(xt, xbuf, 0)
                self._rms(xs, xb, W, sq, b_sq, rstd, b_rstd)
                for dc in range(8):
                    ti, tb = tmp_r.next()
                    k.op("dve", lambda e: e.tensor_tensor(out=tmp[:, ti, 0:W], in0=xs[:, dc, 0:W], in1=rstd[:, 0:W], op=ALU.mult),
                         reads=[xb, b_rstd], writes=[tb])
                    k.op("act", lambda e: e.activation(out=h[:, dc, 0:W], in_=tmp[:, ti, 0:W], func=AF.Identity,
                                                       bias=self.modv(l, 3 * s_idx, dc, v),
                                                       scale=self.pre[:, l, s_idx, v, dc:dc + 1]),
                         reads=[tb], writes=[b_h[dc]])
                if blk + 1 < nblk:
                    self._xload(xt, xbuf, blk + 1)
                for j in range(22):
                    wi, wb = w1_r.next()
                    k.dma("sp", w1[:, wi, :], self.wtile(l, woff_in + j * 128 * 2048, 128, 2048), wb,
                          reads=[self.wall_b[l]], writes=[wb])
                    ba, bba = self.bank(2, 8)
                    bb_, bbb = self.bank(2, 8)
                    for kc in range(8):
                        k.op("pe", lambda e: e.matmul(self.psum[:, ba, 0:W], lhsT=w1[:, wi, kc * 256:kc * 256 + 128],
                                                      rhs=h[:, kc, 0:W], start=(kc == 0), stop=(kc == 7)),
                             reads=[wb, b_h[kc]], writes=[bba], sig=(kc == 7))
                    for kc in range(8):
                        k.op("pe", lambda e: e.matmul(self.psum[:, bb_, 0:W], lhsT=w1[:, wi, kc * 256 + 128:kc * 256 + 256],
                                                      rhs=h[:, kc, 0:W], start=(kc == 0), stop=(kc == 7)),
                             reads=[wb, b_h[kc]], writes=[bbb], sig=(kc == 7))
                    si, sb_ = sa_r.next()
                    k.op("act", lambda e: e.activation(out=sa[:, si, 0:W], in_=self.psum[:, ba, 0:W], func=AF.Silu),
                         reads=[bba], writes=[sb_])
                    k.op("dve", lambda e: e.tensor_tensor(out=hid[:, j, 0:W], in0=sa[:, si, 0:W], in1=self.psum[:, bb_, 0:W], op=ALU.mult),
                         reads=[sb_, bbb], writes=[b_hid[j]])
                for oc in range(8):
                    wi, wb = w2_r.next()
                    k.dma("sp", w2[:, wi, :], self.wtile(l, woff_out + oc * 128 * 2816, 128, 2816), wb,
                          reads=[self.wall_b[l]], writes=[wb])
                    bo, bbo = self.bank(2, 8)
                    for kc in range(22):
                        k.op("pe", lambda e: e.matmul(self.psum[:, bo, 0:W], lhsT=w2[:, wi, kc * 128:(kc + 1) * 128],
                                                      rhs=hid[:, kc, 0:W], start=(kc == 0), stop=(kc == 21)),
                             reads=[wb, b_hid[kc]], writes=[bbo], sig=(kc == 21))
                    k.op("act", lambda e: e.activation(out=y[:, oc, 0:W], in_=self.psum[:, bo, 0:W], func=AF.Copy),
                         reads=[bbo], writes=[b_y[oc]])
                self._post_update(l, s_idx, v, y, b_y, xs, xb, W, sq, b_sq, rstd, b_rstd, tmp, tmp_r)
                dst = self.outT if final else self.xres
                k.dma("pool", dst.ap()[:, :, c0:c0 + W].rearrange("c p t -> p c t"), xs[:, :, 0:W], xb,
                      reads=[xb], writes=[self.xres_b[blk]])
            k.barrier()

    def blkgeom(self, blk):
        return (512, blk * 512) if blk < 8 else (CT, NT + (blk - 8) * CT)

    def _xload(self, xt, xbuf, blk):
        W, c0 = self.blkgeom(blk)
        s = blk % 2
        self.k.dma("sp", xt[:, s, :, 0:W], self.xres.ap()[:, :, c0:c0 + W].rearrange("c p t -> p c t"), xbuf[s],
                   reads=[self.xres_b[blk]], writes=[xbuf[s]])

    def _rms(self, xs, xb, W, sq, b_sq, rstd, b_rstd, n=8):
        k = self.k
        for dc in range(n):
            k.op("pool", lambda e: e.tensor_tensor(out=sq[:, dc, 0:W], in0=xs[:, dc, 0:W], in1=xs[:, dc, 0:W], op=ALU.mult),
                 reads=[xb] if isinstance(xb, Buf) else [xb[dc]], writes=[b_sq[dc]])
        bi, bb = self.bank(0, 2)
        for dc in range(n):
            k.op("pe", lambda e: e.matmul(self.psum[:, bi, 0:W], lhsT=self.ones_bf[:], rhs=sq[:, dc, 0:W],
                                          start=(dc == 0), stop=(dc == n - 1)),
                 reads=[b_sq[dc]], writes=[bb], sig=(dc == n - 1))
        k.op("act", lambda e: e.activation(out=rstd[:, 0:W], in_=self.psum[:, bi, 0:W], func=AF.Sqrt,
                                           bias=self.cst[:, 1:2], scale=1.0 / (128 * n)), reads=[bb], writes=[b_rstd])
        k.op("dve", lambda e: e.reciprocal(out=rstd[:, 0:W], in_=rstd[:, 0:W]), reads=[b_rstd], writes=[b_rstd])

    def _post_update(self, l, s_idx, v, y, b_y, xs, xb, W, sq, b_sq, rstd, b_rstd, tmp, tmp_r):
        k = self.k
        self._rms(y, b_y, W, sq, b_sq, rstd, b_rstd)
        for dc in range(8):
            ti, tb = tmp_r.next()
            k.op("dve", lambda e: e.tensor_tensor(out=tmp[:, ti, 0:W], in0=y[:, dc, 0:W], in1=rstd[:, 0:W], op=ALU.mult),
                 reads=[b_y[dc], b_rstd], writes=[tb])
            k.op("dve", lambda e: e.scalar_tensor_tensor(out=xs[:, dc, 0:W], in0=tmp[:, ti, 0:W],
                                                         scalar=self.post[:, l, s_idx, v, dc:dc + 1], in1=xs[:, dc, 0:W],
                                                         op0=ALU.mult, op1=ALU.add),
                 reads=[tb, xb], writes=[xb])


def _inproj(self, l):
    k, nc = self.k, self.nc
    with ExitStack() as st:
        def sbt(name, shape, dt):
            return st.enter_context(nc.sbuf_tensor(self.un(name), shape, dt))
        xt = sbt("i_x", [128, 2, 8, 512], F32)
        xbuf = [Buf(), Buf()]
        sq = sbt("i_sq", [128, 8, 512], BF16)
        b_sq = [Buf() for _ in range(8)]
        rstd = sbt("i_rstd", [128, 512], F32)
        b_rstd = Buf()
        tmp = sbt("i_tmp", [128, 4, 512], F32)
        tmp_r = Ring(tmp, 4)
        h = sbt("i_h", [128, 8, 512], BF16)
        b_h = [Buf() for _ in range(8)]
        rt = sbt("i_rt", [128, 2, 4, 512], F32)
        b_rt = [Buf(), Buf()]
        wf = sbt("i_wf", [128, 8, 1024], BF16)
        wf_r = Ring(wf, 8)
        wtm = sbt("i_wtm", [128, 4, 4096], BF16)
        b_wtm = Buf()
        og = sbt("i_og", [128, 6, 512], BF16)
        og_r = Ring(og, 6)
        k.dma("sp", wtm[:], self.wtile(l, OFF_WTM, 512, 4096).rearrange("(g p) k -> p g k", p=128), b_wtm,
              reads=[self.wall_b[l]], writes=[b_wtm])

        def fm(ci, W):
            wi, wb = wf_r.next()
            k.dma("sp", wf[:, wi, :], self.wtile(l, OFF_WFM + ci * 128 * 1024, 128, 1024), wb,
                  reads=[self.wall_b[l]], writes=[wb])
            bi, bb = self.bank(2, 8)
            for kc in range(8):
                k.op("pe", lambda e: e.matmul(self.psum[:, bi, 0:W], lhsT=wf[:, wi, kc * 128:(kc + 1) * 128],
                                              rhs=h[:, kc, 0:W], start=(kc == 0), stop=(kc == 7)),
                     reads=[wb, b_h[kc]], writes=[bb], sig=(kc == 7))
            return bi, bb

        for blk in range(10):
            v = BLKV[blk]
            W, c0 = self.blkgeom(blk)
            s = blk % 2
            xs, xb = xt[:, s], xbuf[s]
            bt = blk // 4 if blk < 8 else blk - 8
            t0 = (blk % 4) * 512
            rc0 = t0 if blk < 8 else NB
            if blk == 0:
                self._xload(xt, xbuf, 0)
            k.dma("sp", rt[:, s, :, 0:W], self.ropes.ap()[:, :, rc0:rc0 + W].rearrange("f p t -> p f t"), b_rt[s],
                  writes=[b_rt[s]])
            self._rms(xs, xb, W, sq, b_sq, rstd, b_rstd)
            for dc in range(8):
                ti, tb = tmp_r.next()
                k.op("dve", lambda e: e.tensor_tensor(out=tmp[:, ti, 0:W], in0=xs[:, dc, 0:W], in1=rstd[:, 0:W], op=ALU.mult),
                     reads=[xb, b_rstd], writes=[tb])
                k.op("act", lambda e: e.activation(out=h[:, dc, 0:W], in_=tmp[:, ti, 0:W], func=AF.Identity,
                                                   bias=self.modv(l, 3, dc, v), scale=self.pre[:, l, 1, v, dc:dc + 1]),
                     reads=[tb], writes=[b_h[dc]])
            if blk + 1 < 10:
                self._xload(xt, xbuf, blk + 1)
            pairs = []
            for hh in range(4):
                pairs.append((C_RQ + hh, C_RQS + hh, 0, self.QR.ap()[hh, :, c0:c0 + W], self.scb("QR", blk)))
            for hh in range(4):
                pairs.append((C_RK + hh, C_RKS + hh, 0, self.KR.ap()[hh, :, c0:c0 + W], self.scb("KR", blk)))
            for hh in range(8):
                pairs.append((C_DQ + hh, C_DQS + hh, 2, self.DQ.ap()[hh, :, c0:c0 + W], self.scb("DQ", blk)))
            for hh in range(8):
                if blk < 8:
                    r0 = (bt * 8 + hh) * 128
                    dst = self.DKs.ap()[r0:r0 + 128, t0:t0 + W]
                else:
                    dst = self.DKc.ap()[bt, hh, :, :]
                pairs.append((C_DK + hh, C_DKS + hh, 2, dst, self.scb("DK", blk)))
            for (ca, cbb, ti_, dst, dbuf) in pairs:
                pa, ba = fm(ca, W)
                pb, bb = fm(cbb, W)
                t1, tb1 = tmp_r.next()
                k.op("dve", lambda e: e.tensor_tensor(out=tmp[:, t1, 0:W], in0=self.psum[:, pa, 0:W], in1=rt[:, s, ti_, 0:W], op=ALU.mult),
                     reads=[ba, b_rt[s]], writes=[tb1])
                t2, tb2 = tmp_r.next()
                k.op("dve", lambda e: e.tensor_tensor(out=tmp[:, t2, 0:W], in0=self.psum[:, pb, 0:W], in1=rt[:, s, ti_ + 1, 0:W], op=ALU.mult),
                     reads=[bb, b_rt[s]], writes=[tb2])
                oi, ob = og_r.next()
                k.op("pool", lambda e: e.tensor_tensor(out=og[:, oi, 0:W], in0=tmp[:, t1, 0:W], in1=tmp[:, t2, 0:W], op=ALU.add),
                     reads=[tb1, tb2], writes=[ob])
                k.dma("pool", dst, og[:, oi, 0:W], ob, reads=[ob], writes=[dbuf])
            for (cbase, func, dt_, nm) in ((C_RG, AF.Silu, self.RG, "RG"), (C_GA, AF.Sigmoid, self.GA, "GA"), (C_GB, AF.Sigmoid, self.GB, "GB")):
                for c in range(8):
                    pa, ba = fm(cbase + c, W)
                    oi, ob = og_r.next()
                    k.op("act", lambda e: e.activation(out=og[:, oi, 0:W], in_=self.psum[:, pa, 0:W], func=func),
                         reads=[ba], writes=[ob])
                    k.dma("pool", dt_.ap()[c, :, c0:c0 + W], og[:, oi, 0:W], ob, reads=[ob], writes=[self.scb(nm, blk)])
            for tt in range(W // 128):
                for g in range(4):
                    bi, bb = self.bank(2, 8)
                    for kc in range(8):
                        k.op("pe", lambda e: e.matmul(self.psum[:, bi, :], lhsT=h[:, kc, tt * 128:(tt + 1) * 128],
                                                      rhs=wtm[:, g, kc * 512:(kc + 1) * 512], start=(kc == 0), stop=(kc == 7)),
                             reads=[b_wtm, b_h[kc]], writes=[bb], sig=(kc == 7))
                    oi, ob = og_r.next()
                    if g % 2 == 0:
                        k.op("act", lambda e: e.activation(out=og[:, oi, :], in_=self.psum[:, bi, :], func=AF.Copy),
                             reads=[bb], writes=[ob])
                    else:
                        k.op("dve", lambda e: e.tensor_copy(out=og[:, oi, :], in_=self.psum[:, bi, :]), reads=[bb], writes=[ob])
                    if g < 2:
                        dst = self.RV.ap()[c0 + tt * 128:c0 + (tt + 1) * 128, g * 512:(g + 1) * 512]
                        dbuf = self.scb("RV", blk)
                    elif blk < 8:
                        r0 = bt * NB + t0 + tt * 128
                        dst = self.DVs.ap()[r0:r0 + 128, (g - 2) * 512:(g - 1) * 512]
                        dbuf = self.scb("DV", blk)
                    else:
                        r0 = bt * CT + tt * 128
                        dst = self.DVc.ap()[r0:r0 + 128, (g - 2) * 512:(g - 1) * 512]
                        dbuf = self.scb("DV", blk)
                    k.dma("pool", dst, og[:, oi, :], ob, reads=[ob], writes=[dbuf])
        k.barrier()
        self.b_dkg, self.b_dvg = Buf("DKg"), Buf("DVg")
        self.allgather(self.DKs.ap().rearrange("a (x y) -> (a x) y", y=1024), 4096, self.DKg.ap(), [], [self.b_dkg], q="sp")
        self.allgather(self.DVs.ap(), 4096, self.DVg.ap(), [], [self.b_dvg], q="sp")


def _ret(self, l, last):
    k, nc = self.k, self.nc
    NCH = 18
    with ExitStack() as st:
        def sbt(name, shape, dt):
            return st.enter_context(nc.sbuf_tensor(self.un(name), shape, dt))
        lg = sbt("r_lg", [128, 8], F32)
        dm = sbt("r_dm", [128, 8, 128], F32)
        xi = sbt("r_xi", [128, 8, 128], F32)
        zeta = sbt("r_zeta", [128, 8], F32)
        gch = sbt("r_gch", [128, 8], F32)
        cf = sbt("r_cf", [128, 8, 9], F32)
        sctx = sbt("r_sctx", [128, 16, 256], F32)
        b_sctx = Buf()
        qT = sbt("r_qT", [128, NCH * 128], BF16)
        kT = sbt("r_kT", [128, NCH * 128], BF16)
        k32 = sbt("r_k32", [128, NCH * 128], F32)
        vt = sbt("r_v", [128, NCH, 256], BF16)
        kz = sbt("r_kz", [128, 2, NCH, 128], BF16)
        qx = sbt("r_qx", [128, 2, NCH * 128], BF16)
        S = sbt("r_S", [128, 2, 256], F32)
        Sb = sbt("r_Sb", [128, 2, NCH, 256], BF16)
        sm = sbt("r_sm", [128, 2, 2, 128], BF16)
        Y = sbt("r_Y", [128, 2, NCH * 128], F32)
        Ysq = sbt("r_Ysq", [128, 2, 512], F32)
        gt = sbt("r_gt", [128, 2, 2, NCH * 128], BF16)
        st1 = sbt("r_st", [128, 4, 512], F32)
        ob = sbt("r_ob", [128, 2, 512], BF16)
        ag = sbt("r_ag", [128, 8, 256], F32)
        ast = sbt("r_ast", [128, 256], F32)
        b_c = Buf()
        b_q, b_k, b_k32, b_v, b_kz, b_qx, b_S, b_Sb, b_Y, b_Ysq, b_gt, b_ag, b_ast = [Buf() for _ in range(13)]
        b_sm = [Buf(), Buf()]
        b_st = Buf()
        ob_r = Ring(ob, 2)
        dc8 = self.decay_sb[:, l * 8:(l + 1) * 8]
        k.op("act", lambda e: e.activation(out=lg[:], in_=dc8, func=AF.Exp, scale=-1.0), writes=[b_c])
        k.op("act", lambda e: e.activation(out=lg[:], in_=lg[:], func=AF.Ln, bias=self.cst[:, 0:1], scale=1.0), reads=[b_c], writes=[b_c])
        k.op("dve", lambda e: e.tensor_scalar(out=lg[:], in0=lg[:], scalar1=-1.0, scalar2=None, op0=ALU.mult), reads=[b_c], writes=[b_c])
        for d in range(2):
            for hh in range(4):
                i = d * 4 + hh
                k.op("act", lambda e: e.activation(out=dm[:, i, :], in_=self.misc_sb[:, 2 * d, :], func=AF.Exp, scale=lg[:, i:i + 1]), reads=[b_c], writes=[b_c])
                k.op("dve", lambda e: e.tensor_tensor(out=dm[:, i, :], in0=dm[:, i, :], in1=self.misc_sb[:, 2 * d + 1, :], op=ALU.mult), reads=[b_c], writes=[b_c])
                k.op("act", lambda e: e.activation(out=xi[:, i, :], in_=self.misc_sb[:, 4 + d, :], func=AF.Exp, scale=lg[:, i:i + 1]), reads=[b_c], writes=[b_c])
                k.op("dve", lambda e: e.tensor_scalar(out=xi[:, i, :], in0=xi[:, i, :], scalar1=128.0 ** -0.5, scalar2=None, op0=ALU.mult), reads=[b_c], writes=[b_c])
                k.op("act", lambda e: e.activation(out=zeta[:, i:i + 1], in_=self.misc_sb[:, 6, d:d + 1], func=AF.Exp, scale=lg[:, i:i + 1]), reads=[b_c], writes=[b_c])
                k.op("act", lambda e: e.activation(out=gch[:, i:i + 1], in_=lg[:, i:i + 1], func=AF.Exp, scale=128.0), reads=[b_c], writes=[b_c])
                k.op("act", lambda e: e.activation(out=cf[:, i, :], in_=self.rcoef_sb[:, d, 0, :], func=AF.Exp, scale=lg[:, i:i + 1]), reads=[b_c], writes=[b_c])
                k.op("dve", lambda e: e.tensor_tensor(out=cf[:, i, :], in0=cf[:, i, :], in1=self.rcoef_sb[:, d, 1, :], op=ALU.mult), reads=[b_c], writes=[b_c])

        def cols(bt, n):
            return bt * NB + n * 128 if n < 16 else NT + bt * CT + (n - 16) * 128

        def prep(bt, hh, need_q):
            m0, c0 = bt * NB, NT + bt * CT
            k.dma("sp", kT[:, 0:NB], self.KR.ap()[hh, :, m0:m0 + NB], b_k, writes=[b_k])
            k.dma("sp", kT[:, NB:], self.KR.ap()[hh, :, c0:c0 + CT], b_k, writes=[b_k])
            k.dma("sp", vt[:, 0:16, :], self.RV.ap()[m0:m0 + NB, hh * 256:(hh + 1) * 256].rearrange("(n p) e -> p n e", p=128), b_v, writes=[b_v])
            k.dma("sp", vt[:, 16:18, :], self.RV.ap()[c0:c0 + CT, hh * 256:(hh + 1) * 256].rearrange("(n p) e -> p n e", p=128), b_v, writes=[b_v])
            k.op("pool", lambda e: e.tensor_copy(out=k32[:], in_=kT[:]), reads=[b_k], writes=[b_k32])
            for n in range(NCH):
                bi, bb = self.bank(2, 8)
                k.op("pe", lambda e: e.transpose(self.psum[:, bi, 0:128], k32[:, n * 128:(n + 1) * 128], self.misc_sb[:, 7, :]),
                     reads=[b_k32], writes=[bb])
                for d in range(2):
                    i = d * 4 + hh
                    k.op("act", lambda e: e.activation(out=kz[:, d, n, :], in_=self.psum[:, bi, 0:128], func=AF.Copy, scale=zeta[:, i:i + 1]),
                         reads=[bb], writes=[b_kz])
            if need_q:
                k.dma("sp", qT[:, 0:NB], self.QR.ap()[hh, :, m0:m0 + NB], b_q, writes=[b_q])
                k.dma("sp", qT[:, NB:], self.QR.ap()[hh, :, c0:c0 + CT], b_q, writes=[b_q])
                for d in range(2):
                    i = d * 4 + hh
                    for n in range(NCH):
                        k.op("pool", lambda e: e.tensor_tensor(out=qx[:, d, n * 128:(n + 1) * 128], in0=qT[:, n * 128:(n + 1) * 128],
                                                               in1=xi[:, i, :], op=ALU.mult), reads=[b_q], writes=[b_qx])

        def step(d, hh, n):
            i = d * 4 + hh
            bi, bb = self.bank(2, 8)
            k.op("pe", lambda e: e.matmul(self.psum[:, bi, 0:256], lhsT=kz[:, d, n, :], rhs=vt[:, n, :], start=True, stop=True),
                 reads=[b_kz, b_v], writes=[bb])
            k.op("dve", lambda e: e.scalar_tensor_tensor(out=S[:, d, :], in0=S[:, d, :], scalar=gch[:, i:i + 1], in1=self.psum[:, bi, 0:256],
                                                         op0=ALU.mult, op1=ALU.add), reads=[bb, b_S], writes=[b_S])

        for bt in range(2):
            for hh in range(4):
                prep(bt, hh, False)
                for d in range(2):
                    idx = (bt * 2 + d) * 4 + hh
                    k.op("pool", lambda e: e.memset(S[:, d, :], 0.0), reads=[b_S], writes=[b_S])
                    for n in ((16, 17) if d == 0 else (17, 16)):
                        step(d, hh, n)
                    k.op("act", lambda e: e.activation(out=sctx[:, idx, :], in_=S[:, d, :], func=AF.Copy), reads=[b_S], writes=[b_sctx])
                    k.op("pool", lambda e: e.memset(S[:, d, :], 0.0), reads=[b_S], writes=[b_S])
                    for n in (range(16) if d == 0 else range(15, -1, -1)):
                        step(d, hh, n)
                    k.op("act", lambda e: e.activation(out=ast[:], in_=S[:, d, :], func=AF.Copy), reads=[b_S, b_ast], writes=[b_ast])
                    k.dma("sp", self.Asrc.ap()[idx * 128:(idx + 1) * 128, :], ast[:], b_ast, reads=[b_ast], writes=[self.scb("Asrc", 0)])
        k.barrier()
        b_aall = Buf()
        self.allgather(self.Asrc.ap().bitcast(BF16).rearrange("a (x y) -> (a x) y", y=1024) if False else
                       self.Asrc.ap().bitcast(BF16).rearrange("(a x) y -> a (x y)", x=2), 1024, self.Aall.ap(), [], [b_aall], q="sp")
        aview = self.Aall.ap().bitcast(F32).rearrange("r a (x y) -> r (a x) y", x=2)
        for bt in range(2):
            for hh in range(4):
                prep(bt, hh, True)
                for eh in range(2):
                    c = hh * 2 + eh
                    for gi, src_ in enumerate((self.RG, self.GA)):
                        k.dma("sp", gt[:, gi, eh, 0:NB], src_.ap()[c, :, bt * NB:(bt + 1) * NB], b_gt, writes=[b_gt])
                        k.dma("sp", gt[:, gi, eh, NB:], src_.ap()[c, :, NT + bt * CT:NT + (bt + 1) * CT], b_gt, writes=[b_gt])
                for seq in ((0, 1) if not last else (0,)):
                    chunks = list(range(16)) if seq == 0 else [16, 17]
                    for d in range(2):
                        i = d * 4 + hh
                        idx = (bt * 2 + d) * 4 + hh
                        if seq == 0:
                            k.dma("sp", ag[:], aview[:, idx * 128:(idx + 1) * 128, :].rearrange("r p e -> p r e"), b_ag,
                                  reads=[b_aall], writes=[b_ag])
                            k.op("dve", lambda e: e.tensor_scalar(out=S[:, d, :], in0=sctx[:, idx, :], scalar1=cf[:, i, 8:9], scalar2=None,
                                                                  op0=ALU.mult), reads=[b_sctx, b_S], writes=[b_S])
                            for rr in range(8):
                                k.op("dve", lambda e: e.scalar_tensor_tensor(out=S[:, d, :], in0=ag[:, rr, :], scalar=cf[:, i, rr:rr + 1],
                                                                             in1=S[:, d, :], op0=ALU.mult, op1=ALU.add),
                                     reads=[b_ag, b_S], writes=[b_S])
                        else:
                            k.op("pool", lambda e: e.memset(S[:, d, :], 0.0), reads=[b_S], writes=[b_S])
                    for n in reversed(chunks):
                        k.op("act", lambda e: e.activation(out=Sb[:, 1, n, :], in_=S[:, 1, :], func=AF.Copy), reads=[b_S], writes=[b_Sb])
                        step(1, hh, n)
                    for n in chunks:
                        k.op("act", lambda e: e.activation(out=Sb[:, 0, n, :], in_=S[:, 0, :], func=AF.Copy), reads=[b_S], writes=[b_Sb])
                        bi, bb = self.bank(2, 8)
                        k.op("pe", lambda e: e.matmul(self.psum[:, bi, 0:128], lhsT=kT[:, n * 128:(n + 1) * 128], rhs=qT[:, n * 128:(n + 1) * 128],
                                                      start=True, stop=True), reads=[b_k, b_q], writes=[bb])
                        si = n % 2
                        for d in range(2):
                            k.op("dve", lambda e: e.tensor_tensor(out=sm[:, si, d, :], in0=self.psum[:, bi, 0:128], in1=dm[:, d * 4 + hh, :], op=ALU.mult),
                                 reads=[bb], writes=[b_sm[si]])
                        for eh in range(2):
                            yi, yb = self.bank(2, 8)
                            es = slice(eh * 128, (eh + 1) * 128)
                            ops = [(vt[:, n, es], sm[:, si, 0, :], [b_v, b_sm[si]]), (Sb[:, 0, n, es], qx[:, 0, n * 128:(n + 1) * 128], [b_Sb, b_qx]),
                                   (vt[:, n, es], sm[:, si, 1, :], [b_v, b_sm[si]]), (Sb[:, 1, n, es], qx[:, 1, n * 128:(n + 1) * 128], [b_Sb, b_qx])]
                            for oi_, (lt, rh, rd) in enumerate(ops):
                                k.op("pe", lambda e: e.matmul(self.psum[:, yi, 0:128], lhsT=lt, rhs=rh, start=(oi_ == 0), stop=(oi_ == 3)),
                                     reads=rd, writes=[yb], sig=(oi_ == 3))
                            k.op("act", lambda e: e.activation(out=Y[:, eh, n * 128:(n + 1) * 128], in_=self.psum[:, yi, 0:128], func=AF.Copy),
                                 reads=[yb], writes=[b_Y])
                        step(0, hh, n)
                groups = [(g * 512, 512) for g in range(4)] + ([(NB, CT)] if not last else [])
                for (g0, W) in groups:
                    dcol = bt * NB + g0 if g0 < NB else NT + bt * CT
                    for eh in range(2):
                        k.op("pool", lambda e: e.tensor_tensor(out=Ysq[:, eh, 0:W], in0=Y[:, eh, g0:g0 + W], in1=Y[:, eh, g0:g0 + W], op=ALU.mult),
                             reads=[b_Y], writes=[b_Ysq])
                    b1, bb1 = self.bank(0, 2)
                    b2, bb2 = self.bank(0, 2)
                    for eh in range(2):
                        k.op("pe", lambda e: e.matmul(self.psum[:, b1, 0:W], lhsT=self.ones_f[:], rhs=Y[:, eh, g0:g0 + W], start=(eh == 0), stop=(eh == 1)),
                             reads=[b_Y], writes=[bb1], sig=(eh == 1))
                    for eh in range(2):
                        k.op("pe", lambda e: e.matmul(self.psum[:, b2, 0:W], lhsT=self.ones_f[:], rhs=Ysq[:, eh, 0:W], start=(eh == 0), stop=(eh == 1)),
                             reads=[b_Ysq], writes=[bb2], sig=(eh == 1))
                    mean, var = st1[:, 0, 0:W], st1[:, 1, 0:W]
                    k.op("dve", lambda e: e.tensor_scalar(out=mean, in0=self.psum[:, b1, 0:W], scalar1=1.0 / 256, scalar2=None, op0=ALU.mult),
                         reads=[bb1], writes=[b_st])
                    k.op("dve", lambda e: e.tensor_tensor(out=var, in0=mean, in1=mean, op=ALU.mult), reads=[b_st], writes=[b_st])
                    k.op("dve", lambda e: e.scalar_tensor_tensor(out=var, in0=self.psum[:, b2, 0:W], scalar=1.0 / 256, in1=var,
                                                                 op0=ALU.mult, op1=ALU.subtract), reads=[bb2, b_st], writes=[b_st])
                    k.op("act", lambda e: e.activation(out=var, in_=var, func=AF.Sqrt, bias=self.cst[:, 1:2], scale=1.0), reads=[b_st], writes=[b_st])
                    k.op("dve", lambda e: e.reciprocal(out=var, in_=var), reads=[b_st], writes=[b_st])
                    for eh in range(2):
                        c = hh * 2 + eh
                        t = st1[:, 2 + eh, 0:W]
                        k.op("dve", lambda e: e.tensor_tensor(out=t, in0=Y[:, eh, g0:g0 + W], in1=mean, op=ALU.subtract), reads=[b_Y, b_st], writes=[b_st])
                        k.op("dve", lambda e: e.tensor_tensor(out=t, in0=t, in1=var, op=ALU.mult), reads=[b_st], writes=[b_st])
                        k.op("dve", lambda e: e.scalar_tensor_tensor(out=t, in0=t, scalar=self.gnw_sb[:, l, c:c + 1], in1=gt[:, 0, eh, g0:g0 + W],
                                                                     op0=ALU.mult, op1=ALU.mult), reads=[b_st, b_gt], writes=[b_st])
                        oi, obb = ob_r.next()
                        k.op("dve", lambda e: e.tensor_tensor(out=ob[:, oi, 0:W], in0=t, in1=gt[:, 1, eh, g0:g0 + W], op=ALU.mult),
                             reads=[b_st, b_gt], writes=[obb])
                        k.dma("pool", self.MA.ap()[c, :, dcol:dcol + W], ob[:, oi, 0:W], obb, reads=[obb], writes=[self.scb("MA", 0)])
        k.barrier()


def _diff(self, l, last):
    k, nc = self.k, self.nc
    NKT = 130
    lam_init = 0.8 - 0.6 * math.exp(-0.3 * l)
    with ExitStack() as st:
        def sbt(name, shape, dt):
            return st.enter_context(nc.sbuf_tensor(self.un(name), shape, dt))
        KT = sbt("d_KT", [128, 2, NKT * 128], BF16)
        V = sbt("d_V", [128, 2, NKT, 128], BF16)
        QT = sbt("d_QT", [128, 2, NB + CT], BF16)
        GBt = sbt("d_GB", [128, 2, NB + CT], BF16)
        E = sbt("d_E", [128, 3, 2, 512], BF16)
        E_r = Ring(E, 3)
        lam = sbt("d_lam", [128, 4], F32)
        lt = sbt("d_lt", [128, 256], F32)
        w = sbt("d_w", [128, 6, 512], F32)
        sq = sbt("d_sq", [128, 1, 512], BF16)
        rstd = sbt("d_rstd", [128, 512], F32)
        ob = sbt("d_ob", [128, 2, 512], BF16)
        ob_r = Ring(ob, 2)
        b_KT, b_V, b_QT, b_GB = [Buf(), Buf()], [Buf(), Buf()], [Buf(), Buf()], [Buf(), Buf()]
        b_lam, b_w, b_rstd = Buf(), Buf(), Buf()
        b_sq = [Buf()]
        dl = self.dlam_sb[:, l * 256:(l + 1) * 256]
        k.op("dve", lambda e: e.tensor_tensor(out=lt[:, 0:64], in0=dl[:, 0:64], in1=dl[:, 64:128], op=ALU.mult), writes=[b_lam])
        k.op("dve", lambda e: e.tensor_tensor(out=lt[:, 64:128], in0=dl[:, 128:192], in1=dl[:, 192:256], op=ALU.mult), reads=[b_lam], writes=[b_lam])
        k.op("dve", lambda e: e.reduce_sum(out=lam[:, 0:1], in_=lt[:, 0:64], axis=mybir.AxisListType.X), reads=[b_lam], writes=[b_lam])
        k.op("dve", lambda e: e.reduce_sum(out=lam[:, 1:2], in_=lt[:, 64:128], axis=mybir.AxisListType.X), reads=[b_lam], writes=[b_lam])
        k.op("act", lambda e: e.activation(out=lam[:, 0:2], in_=lam[:, 0:2], func=AF.Exp), reads=[b_lam], writes=[b_lam])
        k.op("dve", lambda e: e.tensor_tensor(out=lam[:, 2:3], in0=lam[:, 1:2], in1=lam[:, 0:1], op=ALU.subtract), reads=[b_lam], writes=[b_lam])
        k.op("dve", lambda e: e.tensor_scalar(out=lam[:, 2:3], in0=lam[:, 2:3], scalar1=-lam_init, scalar2=None, op0=ALU.add), reads=[b_lam], writes=[b_lam])
        k.op("dve", lambda e: e.tensor_scalar(out=lam[:, 3:4], in0=self.subln_sb[:, l:l + 1], scalar1=1.0 - lam_init, scalar2=None, op0=ALU.mult),
             reads=[b_lam], writes=[b_lam])
        dkv = self.DKg.ap().rearrange("r (q x) y -> r q (x y)", x=2)
        it = 0
        for bt in range(2):
            for hh in range(8):
                s = it % 2
                it += 1
                q0 = (bt * 8 + hh) * 128
                k.dma("sp", KT[:, s, 0:CT], self.DKc.ap()[bt, hh, :, :], b_KT[s], reads=[self.scb("DK", 8 + bt)], writes=[b_KT[s]])
                k.dma("sp", KT[:, s, CT:].rearrange("p (r t) -> p r t", r=8), dkv[:, q0:q0 + 128, :].rearrange("r p t -> p r t"), b_KT[s],
                      reads=[self.b_dkg], writes=[b_KT[s]])
                k.dma("sp", V[:, s, 0:2, :], self.DVc.ap()[bt * CT:(bt + 1) * CT, hh * 128:(hh + 1) * 128].rearrange("(n p) e -> p n e", p=128),
                      b_V[s], writes=[b_V[s]])
                for rr in range(8):
                    k.dma("sp", V[:, s, 2 + rr * 16:2 + (rr + 1) * 16, :],
                          self.DVg.ap()[rr, bt * NB:(bt + 1) * NB, hh * 128:(hh + 1) * 128].rearrange("(n p) e -> p n e", p=128),
                          b_V[s], reads=[self.b_dvg], writes=[b_V[s]])
                for (dst_, src_, bb_) in ((QT, self.DQ, b_QT), (GBt, self.GB, b_GB)):
                    k.dma("sp", dst_[:, s, 0:NB], src_.ap()[hh, :, bt * NB:(bt + 1) * NB], bb_[s], writes=[bb_[s]])
                    k.dma("sp", dst_[:, s, NB:], src_.ap()[hh, :, NT + bt * CT:NT + (bt + 1) * CT], bb_[s], writes=[bb_[s]])
                qblocks = [(g * 512, 512, NKT) for g in range(4)] + ([(NB, CT, 2)] if not last else [])
                for (g0, W, nkt) in qblocks:
                    dcol = bt * NB + g0 if g0 < NB else NT + bt * CT
                    for kt in range(nkt):
                        sa = 2 * (kt % 2)
                        for mp in range(2):
                            ps_ = slice(mp * 64, (mp + 1) * 64)
                            k.op("pe", lambda e: e.matmul(self.psum[:, sa + mp, 0:W], lhsT=KT[ps_, s, kt * 128:(kt + 1) * 128], rhs=QT[ps_, s, g0:g0 + W],
                                                          start=True, stop=True), reads=[b_KT[s], b_QT[s]], writes=[self.pb[sa + mp]])
                        ei, eb = E_r.next()
                        k.op("act", lambda e: e.activation(out=E[:, ei, :, 0:W], in_=self.psum[:, sa:sa + 2, 0:W], func=AF.Exp, scale=0.125),
                             reads=[self.pb[sa], self.pb[sa + 1]], writes=[eb])
                        for mp in range(2):
                            k.op("pe", lambda e: e.matmul(self.psum[:, 4 + mp, 0:W], lhsT=V[:, s, kt, :], rhs=E[:, ei, mp, 0:W],
                                                          start=(kt == 0), stop=(kt == nkt - 1)), reads=[b_V[s], eb], writes=[self.pb[4 + mp]],
                                 sig=(kt == nkt - 1))
                            k.op("pe", lambda e: e.matmul(self.psum[:, 6 + mp, 0:W], lhsT=self.ones_bf[:], rhs=E[:, ei, mp, 0:W],
                                                          start=(kt == 0), stop=(kt == nkt - 1)), reads=[eb], writes=[self.pb[6 + mp]],
                                 sig=True)
                    for mp in range(2):
                        k.op("dve", lambda e: e.reciprocal(out=w[:, mp, 0:W], in_=self.psum[:, 6 + mp, 0:W]), reads=[self.pb[6 + mp]], writes=[b_w])
                        k.op("dve", lambda e: e.tensor_tensor(out=w[:, 2 + mp, 0:W], in0=self.psum[:, 4 + mp, 0:W], in1=w[:, mp, 0:W], op=ALU.mult),
                             reads=[self.pb[4 + mp], b_w], writes=[b_w])
                    k.op("dve", lambda e: e.scalar_tensor_tensor(out=w[:, 4, 0:W], in0=w[:, 3, 0:W], scalar=lam[:, 2:3], in1=w[:, 2, 0:W],
                                                                 op0=ALU.mult, op1=ALU.add), reads=[b_w, b_lam], writes=[b_w])
                    self._rms(w[:, 4:5], b_w, W, sq, b_sq, rstd, b_rstd, n=1)
                    k.op("dve", lambda e: e.tensor_tensor(out=w[:, 5, 0:W], in0=w[:, 4, 0:W], in1=rstd[:, 0:W], op=ALU.mult),
                         reads=[b_w, b_rstd], writes=[b_w])
                    oi, obb = ob_r.next()
                    k.op("dve", lambda e: e.scalar_tensor_tensor(out=ob[:, oi, 0:W], in0=w[:, 5, 0:W], scalar=lam[:, 3:4], in1=GBt[:, s, g0:g0 + W],
                                                                 op0=ALU.mult, op1=ALU.mult), reads=[b_w, b_lam, b_GB[s]], writes=[obb])
                    k.dma("pool", self.MB.ap()[hh, :, dcol:dcol + W], ob[:, oi, 0:W], obb, reads=[obb], writes=[self.scb("MB", 0)])
        k.barrier()


def _oproj(self, l, last):
    k, nc = self.k, self.nc
    with ExitStack() as st:
        def sbt(name, shape, dt):
            return st.enter_context(nc.sbuf_tensor(self.un(name), shape, dt))
        xt = sbt("o_x", [128, 2, 8, 512], F32)
        xbuf = [Buf(), Buf()]
        ma = sbt("o_ma", [128, 2, 8, 512], BF16)
        mb = sbt("o_mb", [128, 2, 8, 512], BF16)
        b_ma, b_mb = [Buf(), Buf()], [Buf(), Buf()]
        mg = sbt("o_mg", [128, 8, 512], BF16)
        b_mg = [Buf() for _ in range(8)]
        wo = sbt("o_wo", [128, 8, 1024], BF16)
        b_wo = Buf()
        y = sbt("o_y", [128, 8, 512], F32)
        b_y = [Buf() for _ in range(8)]
        sq = sbt("o_sq", [128, 8, 512], BF16)
        b_sq = [Buf() for _ in range(8)]
        rstd = sbt("o_rstd", [128, 512], F32)
        b_rstd = Buf()
        tmp = sbt("o_tmp", [128, 2, 512], F32)
        tmp_r = Ring(tmp, 2)
        k.dma("sp", wo[:], self.wtile(l, OFF_WO, 1024, 1024).rearrange("(o p) k -> p o k", p=128), b_wo, reads=[self.wall_b[l]], writes=[b_wo])
        nblk = 8 if last else 10
        for blk in range(nblk):
            v = BLKV[blk]
            W, c0 = self.blkgeom(blk)
            s = blk % 2
            xs, xb = xt[:, s], xbuf[s]
            self._xload(xt, xbuf, blk)
            k.dma("sp", ma[:, s, :, 0:W], self.MA.ap()[:, :, c0:c0 + W].rearrange("c p t -> p c t"), b_ma[s], reads=[self.scb("MA", 0)], writes=[b_ma[s]])
            k.dma("sp", mb[:, s, :, 0:W], self.MB.ap()[:, :, c0:c0 + W].rearrange("c p t -> p c t"), b_mb[s], reads=[self.scb("MB", 0)], writes=[b_mb[s]])
            for dc in range(8):
                k.op("pool", lambda e: e.tensor_tensor(out=mg[:, dc, 0:W], in0=ma[:, s, dc, 0:W], in1=mb[:, s, dc, 0:W], op=ALU.add),
                     reads=[b_ma[s], b_mb[s]], writes=[b_mg[dc]])
            for oc in range(8):
                bo, bbo = self.bank(2, 8)
                for kc in range(8):
                    k.op("pe", lambda e: e.matmul(self.psum[:, bo, 0:W], lhsT=wo[:, oc, kc * 128:(kc + 1) * 128], rhs=mg[:, kc, 0:W],
                                                  start=(kc == 0), stop=(kc == 7)), reads=[b_wo, b_mg[kc]], writes=[bbo], sig=(kc == 7))
                k.op("act", lambda e: e.activation(out=y[:, oc, 0:W], in_=self.psum[:, bo, 0:W], func=AF.Copy), reads=[bbo], writes=[b_y[oc]])
            self._post_update(l, 1, v, y, b_y, xs, xb, W, sq, b_sq, rstd, b_rstd, tmp, tmp_r)
            k.dma("pool", self.xres.ap()[:, :, c0:c0 + W].rearrange("c p t -> p c t"), xs[:, :, 0:W], xb, reads=[xb], writes=[self.xres_b[blk]])
        k.barrier()


Prog._inproj, Prog._ret, Prog._diff, Prog._oproj = _inproj, _ret, _diff, _oproj


def build_program(**kw):
    p = Prog(**kw)
    return p.build(), p


def kernel(**inputs):
    maps = prep_inputs(inputs)
    nc, _ = build_program()
    res = run_bass_kernel_spmd(nc, maps, core_ids=list(range(NCORES)))
    out = np.empty((2, T, D), np.float32)
    for core in range(NCORES):
        o = res.results[core]["outT"].reshape(D, NT)
        out[0, core * NB:(core + 1) * NB, :] = o[:, :NB].T
        out[1, core * NB:(core + 1) * NB, :] = o[:, NB:].T
    return out
```
